# Optimizing a Trainium2 kernel written in Bass

```python
import math
import jax, jax.numpy as jnp
from jax import lax
import numpy as np

D_MODEL = 1024
BATCH = 16
SEQ = 2048
DEPTH = 2

MIX_WIDTH = D_MODEL
MLA_WIDTH = D_MODEL // 2
SSM_WIDTH = MIX_WIDTH - MLA_WIDTH
V_HEAD = 64
N_HEADS = MLA_WIDTH // V_HEAD
QK_NOPE = 64
QK_ROPE = 32
Q_LORA = D_MODEL // 4
KV_LORA = D_MODEL // 8
ROPE_BASE = 10000.0
Q_BLOCK = 128
ATTN_SCALE = 1.0 / math.sqrt(QK_NOPE + QK_ROPE)
SSM_CH = 16
SSM_GROUPS = SSM_WIDTH // SSM_CH
SSM_STATE = 64
STEP_MIN = 0.001
STEP_MAX = 0.1
OFF_KV = Q_LORA
OFF_KR = OFF_KV + KV_LORA
OFF_SSM = OFF_KR + QK_ROPE
IN_WIDTH = OFF_SSM + SSM_WIDTH
N_EXPERT_GROUPS = 4
EXPERTS_PER_GROUP = 8
N_EXPERTS = N_EXPERT_GROUPS * EXPERTS_PER_GROUP
TOP_K = 2
D_EXPERT = D_MODEL // 4
DISPATCH_BLOCK = 128
PLE_DIM = 256
EPS = 1e-6

kernel_name = "hymba_mla_s5_hiermoe_ple"


def rmsnorm(x, g):
    xf = x.astype(jnp.float32)
    y = xf * lax.rsqrt(jnp.mean(xf * xf, axis=-1, keepdims=True) + EPS)
    return (y * g.astype(jnp.float32)).astype(x.dtype)


def rope_tables(positions):
    half = QK_ROPE // 2
    inv_freq = ROPE_BASE ** (-jnp.arange(half, dtype=jnp.float32) / half)
    ang = positions.astype(jnp.float32)[..., None] * inv_freq
    return jnp.cos(ang)[:, :, None, :], jnp.sin(ang)[:, :, None, :]


def apply_rope(x, cos, sin):
    half = QK_ROPE // 2
    xf = x.astype(jnp.float32)
    x1, x2 = xf[..., :half], xf[..., half:]
    return jnp.concatenate([x1 * cos - x2 * sin, x1 * sin + x2 * cos], axis=-1).astype(x.dtype)


def causal_attention(q, k, v):
    bsz, seq, heads, dqk = q.shape
    nb = seq // Q_BLOCK
    qb = q.reshape(bsz, nb, Q_BLOCK, heads, dqk).transpose(1, 0, 2, 3, 4)
    kpos = jnp.arange(seq)

    def one_block(args):
        qi, bi = args
        s = jnp.einsum('bqhd,bkhd->bhqk', qi, k).astype(jnp.float32) * ATTN_SCALE
        qpos = bi * Q_BLOCK + jnp.arange(Q_BLOCK)
        s = jnp.where(kpos[None, :] <= qpos[:, None], s, -1e30)
        pr = jax.nn.softmax(s, axis=-1).astype(v.dtype)
        return jnp.einsum('bhqk,bkhd->bqhd', pr, v)

    ob = lax.map(one_block, (qb, jnp.arange(nb)))
    return ob.transpose(1, 0, 2, 3, 4).reshape(bsz, seq, heads * v.shape[-1])


def mla_group(q_lat, kv_lat, k_rope_in, cos, sin, g_q, w_q_up, g_kv, w_kv_up):
    bsz, seq, _ = q_lat.shape
    q = (rmsnorm(q_lat, g_q) @ w_q_up).reshape(bsz, seq, N_HEADS, QK_NOPE + QK_ROPE)
    q = jnp.concatenate([q[..., :QK_NOPE], apply_rope(q[..., QK_NOPE:], cos, sin)], axis=-1)
    kv = (rmsnorm(kv_lat, g_kv) @ w_kv_up).reshape(bsz, seq, N_HEADS, QK_NOPE + V_HEAD)
    k_pe = apply_rope(k_rope_in[:, :, None, :], cos, sin)
    k = jnp.concatenate([kv[..., :QK_NOPE],
                         jnp.broadcast_to(k_pe, (bsz, seq, N_HEADS, QK_ROPE))], axis=-1)
    v = kv[..., QK_NOPE:]
    return causal_attention(q, k, v)


def _lin_rec_combine(e1, e2):
    a1, b1 = e1
    a2, b2 = e2
    return a2 * a1, a2 * b1 + b2


def s5_group(u, a_re, a_im, b_re, b_im, c_re, c_im, d, log_step, w_glu, b_glu):
    f32 = jnp.float32
    bsz, seq, _ = u.shape
    u32 = u.astype(f32).reshape(bsz, seq, SSM_GROUPS, SSM_CH)
    lam = lax.complex(a_re.astype(f32), a_im.astype(f32))
    delta = jnp.exp(log_step.astype(f32))[:, None]
    lam_bar = jnp.exp(lam * delta)
    b_fac = (lam_bar - 1.0) / lam
    bu = lax.complex(jnp.einsum('blgh,gph->blgp', u32, b_re.astype(f32)),
                     jnp.einsum('blgh,gph->blgp', u32, b_im.astype(f32))) * b_fac
    a = jnp.broadcast_to(lam_bar, (1, seq) + lam_bar.shape)
    _, states = lax.associative_scan(_lin_rec_combine, (a, bu), axis=1)
    y = (jnp.einsum('blgp,ghp->blgh', jnp.real(states), c_re.astype(f32))
         - jnp.einsum('blgp,ghp->blgh', jnp.imag(states), c_im.astype(f32))
         + d.astype(f32) * u32)
    y = jax.nn.gelu(y.reshape(bsz, seq, SSM_WIDTH))
    y = y * jax.nn.sigmoid(y @ w_glu.astype(f32) + b_glu.astype(f32))
    return y.astype(u.dtype)


def hier_moe(xn, w_gr, b_gr, w_er, b_er, w_g, w_u, w_d):
    bsz, seq, dm = xn.shape
    t = bsz * seq
    xf = xn.reshape(t, dm)
    g_prob = jax.nn.softmax((xf @ w_gr).astype(jnp.float32) + b_gr.astype(jnp.float32), axis=-1)
    g_top_p, g_top = lax.top_k(g_prob, 1)
    e_logits = ((xf @ w_er).astype(jnp.float32) + b_er.astype(jnp.float32)).reshape(t, N_EXPERT_GROUPS, EXPERTS_PER_GROUP)
    e_sel = jnp.take_along_axis(e_logits, g_top[:, :, None], axis=1)[:, 0]
    e_top_l, e_top = lax.top_k(e_sel, TOP_K)
    weights = g_top_p * jax.nn.softmax(e_top_l, axis=-1)
    expert_id = g_top * EXPERTS_PER_GROUP + e_top
    flat_e = expert_id.reshape(-1)
    flat_tok = jnp.repeat(jnp.arange(t, dtype=jnp.int32), TOP_K, total_repeat_length=t * TOP_K)
    flat_w = weights.reshape(-1)
    order = jnp.argsort(flat_e)
    se, stok, sw = flat_e[order], flat_tok[order], flat_w[order]
    counts = jnp.bincount(flat_e, length=N_EXPERTS)
    starts = jnp.cumsum(counts) - counts
    pcounts = (counts + DISPATCH_BLOCK - 1) // DISPATCH_BLOCK * DISPATCH_BLOCK
    pends = jnp.cumsum(pcounts)
    pstarts = pends - pcounts
    dest = pstarts[se] + (jnp.arange(t * TOP_K) - starts[se])
    n_slots = t * TOP_K + N_EXPERTS * DISPATCH_BLOCK
    n_blocks = n_slots // DISPATCH_BLOCK
    xbuf = jnp.zeros((n_slots, dm), xn.dtype).at[dest].set(xf[stok])
    block_e = jnp.minimum(jnp.searchsorted(pends, jnp.arange(n_blocks) * DISPATCH_BLOCK, side='right'),
                          N_EXPERTS - 1)

    def expert_block(args):
        xb, e = args
        hdn = jax.nn.silu(xb @ w_g[e]) * (xb @ w_u[e])
        return hdn @ w_d[e]

    ybuf = lax.map(expert_block, (xbuf.reshape(n_blocks, DISPATCH_BLOCK, dm), block_e)).reshape(n_slots, dm)
    y = jax.ops.segment_sum(ybuf[dest] * sw[:, None].astype(xn.dtype), stok, num_segments=t)
    return y.reshape(bsz, seq, dm)


def setup_inputs(seed: int = 0) -> dict:
    key = jax.random.key(seed)
    ks = jax.random.split(key, 40)
    nrm = lambda k, shape, s: jax.random.normal(k, shape, jnp.float32) * s
    gain = lambda k, n: 1.0 + 0.01 * jax.random.normal(k, (DEPTH, n), jnp.float32)
    L = DEPTH
    a_im = jnp.pi * jnp.arange(SSM_STATE, dtype=jnp.float32)
    return {
        "x": nrm(ks[0], (BATCH, SEQ, D_MODEL), 1.0),
        "p": nrm(ks[1], (DEPTH, BATCH, SEQ, PLE_DIM), 1.0),
        "positions": jnp.broadcast_to(jnp.arange(SEQ, dtype=jnp.int32), (BATCH, SEQ)),
        "g_mix_norm": gain(ks[2], D_MODEL),
        "w_in": nrm(ks[3], (L, D_MODEL, IN_WIDTH), D_MODEL ** -0.5),
        "g_q_lat": gain(ks[4], Q_LORA),
        "w_q_up": nrm(ks[5], (L, Q_LORA, N_HEADS * (QK_NOPE + QK_ROPE)), Q_LORA ** -0.5),
        "g_kv_lat": gain(ks[6], KV_LORA),
        "w_kv_up": nrm(ks[7], (L, KV_LORA, N_HEADS * (QK_NOPE + V_HEAD)), KV_LORA ** -0.5),
        "ssm_a_re": -0.5 + 0.01 * jax.random.normal(ks[8], (L, SSM_GROUPS, SSM_STATE), jnp.float32),
        "ssm_a_im": a_im + 0.01 * jax.random.normal(ks[9], (L, SSM_GROUPS, SSM_STATE), jnp.float32),
        "ssm_b_re": nrm(ks[10], (L, SSM_GROUPS, SSM_STATE, SSM_CH), (2 * SSM_CH) ** -0.5),
        "ssm_b_im": nrm(ks[11], (L, SSM_GROUPS, SSM_STATE, SSM_CH), (2 * SSM_CH) ** -0.5),
        "ssm_c_re": nrm(ks[12], (L, SSM_GROUPS, SSM_CH, SSM_STATE), SSM_STATE ** -0.5),
        "ssm_c_im": nrm(ks[13], (L, SSM_GROUPS, SSM_CH, SSM_STATE), SSM_STATE ** -0.5),
        "ssm_d": nrm(ks[14], (L, SSM_GROUPS, SSM_CH), 1.0),
        "ssm_log_step": jax.random.uniform(ks[15], (L, SSM_GROUPS), jnp.float32,
                                           math.log(STEP_MIN), math.log(STEP_MAX)),
        "w_glu": nrm(ks[16], (L, SSM_WIDTH, SSM_WIDTH), SSM_WIDTH ** -0.5),
        "b_glu": nrm(ks[17], (L, SSM_WIDTH), 0.01),
        "g_attn_out": gain(ks[18], MLA_WIDTH),
        "g_ssm_out": gain(ks[19], SSM_WIDTH),
        "w_out": nrm(ks[20], (L, MIX_WIDTH, D_MODEL), MIX_WIDTH ** -0.5),
        "g_moe_norm": gain(ks[21], D_MODEL),
        "w_group_router": nrm(ks[22], (L, D_MODEL, N_EXPERT_GROUPS), D_MODEL ** -0.5),
        "b_group_router": nrm(ks[23], (L, N_EXPERT_GROUPS), 0.01),
        "w_expert_router": nrm(ks[24], (L, D_MODEL, N_EXPERTS), D_MODEL ** -0.5),
        "b_expert_router": nrm(ks[25], (L, N_EXPERTS), 0.01),
        "w_exp_gate": nrm(ks[26], (L, N_EXPERTS, D_MODEL, D_EXPERT), D_MODEL ** -0.5),
        "w_exp_up": nrm(ks[27], (L, N_EXPERTS, D_MODEL, D_EXPERT), D_MODEL ** -0.5),
        "w_exp_down": nrm(ks[28], (L, N_EXPERTS, D_EXPERT, D_MODEL), D_EXPERT ** -0.5),
        "g_ple_norm": gain(ks[29], D_MODEL),
        "w_ple_gate": nrm(ks[30], (L, D_MODEL, D_MODEL), D_MODEL ** -0.5),
        "w_ple_proj": nrm(ks[31], (L, PLE_DIM, D_MODEL), PLE_DIM ** -0.5),
        "g_final": 1.0 + 0.01 * jax.random.normal(ks[32], (D_MODEL,), jnp.float32),
    }


def reference(x, p, positions, g_mix_norm, w_in, g_q_lat, w_q_up, g_kv_lat, w_kv_up,
              ssm_a_re, ssm_a_im, ssm_b_re, ssm_b_im, ssm_c_re, ssm_c_im, ssm_d, ssm_log_step,
              w_glu, b_glu, g_attn_out, g_ssm_out, w_out, g_moe_norm,
              w_group_router, b_group_router, w_expert_router, b_expert_router,
              w_exp_gate, w_exp_up, w_exp_down, g_ple_norm, w_ple_gate, w_ple_proj, g_final):
    cos, sin = rope_tables(positions)
    h = x
    for i in range(DEPTH):
        z = rmsnorm(h, g_mix_norm[i]) @ w_in[i]
        attn = mla_group(z[..., :OFF_KV], z[..., OFF_KV:OFF_KR], z[..., OFF_KR:OFF_SSM], cos, sin,
                         g_q_lat[i], w_q_up[i], g_kv_lat[i], w_kv_up[i])
        ssm = s5_group(z[..., OFF_SSM:], ssm_a_re[i], ssm_a_im[i], ssm_b_re[i], ssm_b_im[i],
                       ssm_c_re[i], ssm_c_im[i], ssm_d[i], ssm_log_step[i], w_glu[i], b_glu[i])
        mix = jnp.concatenate([rmsnorm(attn, g_attn_out[i]), rmsnorm(ssm, g_ssm_out[i])], axis=-1)
        h = h + mix @ w_out[i]
        h = h + hier_moe(rmsnorm(h, g_moe_norm[i]), w_group_router[i], b_group_router[i],
                         w_expert_router[i], b_expert_router[i],
                         w_exp_gate[i], w_exp_up[i], w_exp_down[i])
        gate = jax.nn.sigmoid(rmsnorm(h, g_ple_norm[i]) @ w_ple_gate[i])
        h = h + (p[i] @ w_ple_proj[i]) * gate
    return rmsnorm(h, g_final)
```

```python
import math
import numpy as np
import concourse.bass as bass
import concourse.mybir as mybir
from concourse.bass_utils import run_bass_kernel_spmd

F32 = mybir.dt.float32
BF16 = mybir.dt.bfloat16
I32 = mybir.dt.int32
AF = mybir.ActivationFunctionType
ALU = mybir.AluOpType
AX = mybir.AxisListType

S = 2048
NT = 16
D = 1024
KT = 8
NL = 2
NSEQ = 2
NCORES = 8
MAGIC = 12582912.0
TWO_PI = 2.0 * math.pi
ATTN_SCALE = 1.0 / math.sqrt(96.0)
EPS = 1e-6
NPAR = 96


class Tile:
    __slots__ = ("name", "w_eng", "w_dma", "r_eng", "r_dma", "sem", "dma_count", "excl", "fresh")

    def __init__(self, name):
        self.name = name
        self.excl = False
        self.fresh = False
        self.w_eng = {}
        self.w_dma = []
        self.r_eng = {}
        self.r_dma = []
        self.sem = None
        self.dma_count = 0


class V:
    __slots__ = ("tiles", "ap")

    def __init__(self, tiles, ap):
        self.tiles = tiles
        self.ap = ap

    def __getitem__(self, k):
        return V(self.tiles, self.ap[k])

    def re(self, pat, **kw):
        return V(self.tiles, self.ap.rearrange(pat, **kw))

    def bc(self, shape):
        return V(self.tiles, self.ap.broadcast_to(list(shape)))

    def un(self, axis):
        return V(self.tiles, self.ap.unsqueeze(axis))

    def cast(self, dt):
        return V(self.tiles, self.ap.bitcast(dt))

    def on(self, *tiles):
        return V(tuple(t for v in tiles for t in v.tiles), self.ap)

    @property
    def shape(self):
        return tuple(self.ap.shape)


class Instr:
    __slots__ = ("eng", "fn", "is_dma", "idx", "waits", "signals", "sigcount", "clock", "dma_tile", "dma_count")


class Cut(Exception):
    pass


class SemRec:
    __slots__ = ("sem", "count")

    def __init__(self, sem):
        self.sem = sem
        self.count = 0


class Prog:
    ENGS = ("pe", "act", "dve", "pool", "sp")
    budget = None

    def __init__(self, nc):
        self.nc = nc
        self.lists = {e: [] for e in self.ENGS}
        self.clock = {e: {} for e in self.ENGS}
        self.dma_seen = {e: {} for e in self.ENGS}
        self.semtab = {}
        self.ntiles = 0

    def tile(self, name, ap):
        self.ntiles += 1
        return V((Tile(name),), ap)

    @staticmethod
    def inherit(new, olds):
        nt = new.tiles[0]
        nt.fresh = True
        for o in olds:
            for ot in o.tiles:
                if ot is nt:
                    continue
                for src, dst in ((ot.w_eng, nt.w_eng), (ot.r_eng, nt.r_eng)):
                    for e, ins in src.items():
                        if e not in dst or dst[e].idx < ins.idx:
                            dst[e] = ins
                for ins in ot.w_dma:
                    if ins not in nt.w_dma:
                        nt.w_dma.append(ins)
                for ins in ot.r_dma:
                    if ins not in nt.r_dma:
                        nt.r_dma.append(ins)

    def add(self, eng, fn, reads=(), writes=(), is_dma=False, acc=False, dma_tile=None):
        if self.budget is not None:
            if self.budget <= 0:
                raise Cut()
            self.budget -= 1
        I = Instr()
        I.eng = eng
        I.fn = fn
        I.is_dma = is_dma
        I.idx = len(self.lists[eng])
        I.waits = []
        I.signals = False
        I.sigcount = 0
        I.dma_tile = None
        I.dma_count = 0
        deps = []
        wt = []
        for v in writes:
            for t in v.tiles:
                if t not in wt:
                    wt.append(t)
        rt = []
        for v in reads:
            if v is None:
                continue
            for t in v.tiles:
                if t not in rt:
                    rt.append(t)
        for t in rt:
            deps.extend(t.w_eng.values())
            deps.extend(t.w_dma)
        for t in rt + wt:
            if t.excl:
                for e_, ins_ in t.w_eng.items():
                    if e_ != eng:
                        deps.append(ins_)
                for e_, ins_ in t.r_eng.items():
                    if e_ != eng:
                        deps.append(ins_)
        newgen = {}
        for t in wt:
            has_readers = bool(t.r_eng or t.r_dma) or t.fresh
            t.fresh = False
            newgen[id(t)] = has_readers or not acc
            if has_readers or not acc:
                deps.extend(t.r_eng.values())
                deps.extend(t.r_dma)
                deps.extend(t.w_eng.values())
                deps.extend(t.w_dma)
        clock = self.clock[eng]
        seen = self.dma_seen[eng]
        best = {}
        for d in deps:
            if d.is_dma:
                key = id(d.dma_tile)
                if seen.get(key, 0) >= d.dma_count:
                    continue
                seen[key] = d.dma_count
                I.waits.append(d)
                for k, v in d.clock.items():
                    if clock.get(k, -1) < v:
                        clock[k] = v
            else:
                if eng == "pe" and d.eng == "pe" and not is_dma:
                    continue
                if clock.get(d.eng, -1) >= d.idx:
                    continue
                if d.eng not in best or best[d.eng].idx < d.idx:
                    best[d.eng] = d
        for d in best.values():
            if clock.get(d.eng, -1) >= d.idx:
                continue
            I.waits.append(d)
            d.signals = True
            clock[d.eng] = d.idx
            for k, v in d.clock.items():
                if clock.get(k, -1) < v:
                    clock[k] = v
        I.clock = dict(clock)
        if is_dma:
            t = dma_tile.tiles[0]
            rec = self.semtab.get(t.name)
            if rec is None:
                rec = self.semtab[t.name] = SemRec(self.nc.alloc_semaphore("d_" + t.name))
            rec.count += 16
            I.dma_tile = rec
            I.dma_count = rec.count
        for t in wt:
            if newgen[id(t)]:
                t.w_eng = {}
                t.w_dma = []
                t.r_eng = {}
                t.r_dma = []
            if is_dma:
                t.w_dma.append(I)
            else:
                t.w_eng[eng] = I
        for t in rt:
            if t in wt:
                continue
            if is_dma:
                t.r_dma.append(I)
            else:
                t.r_eng[eng] = I
        self.lists[eng].append(I)
        return I

    def mm(self, out, lhsT, rhs, start=True, stop=True):
        o, a, b = out.ap, lhsT.ap, rhs.ap
        self.add("pe", lambda e: e.matmul(o, a, b, start=start, stop=stop), reads=(lhsT, rhs), writes=(out,), acc=not start)

    def tr(self, out, in_, ident, acc=False):
        o, a, b = out.ap, in_.ap, ident.ap
        self.add("pe", lambda e: e.transpose(o, a, b), reads=(in_, ident), writes=(out,), acc=acc)

    def act(self, out, in_, func, bias=None, scale=None, accum=None, acc=False):
        kw = {}
        rd = [in_]
        wr = [out]
        if bias is not None:
            if isinstance(bias, V):
                kw["bias"] = bias.ap
                rd.append(bias)
            else:
                kw["bias"] = bias
        if scale is not None:
            if isinstance(scale, V):
                kw["scale"] = scale.ap
                rd.append(scale)
            else:
                kw["scale"] = scale
        if accum is not None:
            kw["accum_out"] = accum.ap
            wr.append(accum)
        o, a = out.ap, in_.ap
        self.add("act", lambda e: e.activation(out=o, in_=a, func=func, **kw), reads=rd, writes=wr, acc=acc)

    def tt(self, eng, out, a, b, op, acc=False):
        o, x, y = out.ap, a.ap, b.ap
        self.add(eng, lambda e: e.tensor_tensor(o, x, y, op), reads=(a, b), writes=(out,), acc=acc)

    def ts(self, eng, out, a, s1, s2, op0, op1=None, acc=False):
        rd = [a]
        x1 = s1
        x2 = s2
        if isinstance(s1, V):
            rd.append(s1)
            x1 = s1.ap
        if isinstance(s2, V):
            rd.append(s2)
            x2 = s2.ap
        o, x = out.ap, a.ap
        if op1 is None:
            self.add(eng, lambda e: e.tensor_scalar(o, x, x1, None, op0), reads=rd, writes=(out,), acc=acc)
        else:
            self.add(eng, lambda e: e.tensor_scalar(o, x, x1, x2, op0, op1), reads=rd, writes=(out,), acc=acc)

    def stt(self, eng, out, in0, scalar, in1, op0, op1, acc=False):
        rd = [in0, in1]
        sc = scalar
        if isinstance(scalar, V):
            rd.append(scalar)
            sc = scalar.ap
        o, x, y = out.ap, in0.ap, in1.ap
        self.add(eng, lambda e: e.scalar_tensor_tensor(o, x, sc, y, op0, op1), reads=rd, writes=(out,), acc=acc)

    def copy(self, eng, out, in_, acc=False):
        o, x = out.ap, in_.ap
        if eng == "act":
            self.add(eng, lambda e: e.activation(out=o, in_=x, func=AF.Copy), reads=(in_,), writes=(out,), acc=acc)
        else:
            self.add(eng, lambda e: e.tensor_copy(o, x), reads=(in_,), writes=(out,), acc=acc)

    def memset(self, eng, out, val, acc=False):
        o = out.ap
        self.add(eng, lambda e: e.memset(o, val), writes=(out,), acc=acc)

    def recip(self, out, in_):
        o, x = out.ap, in_.ap
        self.add("dve", lambda e: e.reciprocal(o, x), reads=(in_,), writes=(out,))

    def reduce(self, eng, out, in_, op):
        o, x = out.ap, in_.ap
        self.add(eng, lambda e: e.tensor_reduce(o, x, AX.X, op), reads=(in_,), writes=(out,))

    def scan(self, out, d0, d1, init):
        rd = [d0, d1]
        ini = init
        if isinstance(init, V):
            rd.append(init)
            ini = init.ap
        o, x, y = out.ap, d0.ap, d1.ap
        self.add("dve", lambda e: e.tensor_tensor_scan(o, x, y, ini, ALU.mult, ALU.add), reads=rd, writes=(out,))

    def dma(self, eng, out, in_, sem_tile=None, acc=True):
        o, x = out.ap, in_.ap
        st = sem_tile if sem_tile is not None else out
        self.add(eng, lambda e: e.dma_start(out=o, in_=x), reads=(in_,), writes=(out,), is_dma=True, acc=acc, dma_tile=st)

    def emit(self):
        nc = self.nc
        sems = {e: nc.alloc_semaphore("s_" + e) for e in self.ENGS}
        for e in self.ENGS:
            c = 0
            for I in self.lists[e]:
                if I.signals and not I.is_dma:
                    c += 1
                    I.sigcount = c
        lists = self.lists

        def run(e, name):
            for I in lists[name]:
                for d in I.waits:
                    if d.is_dma:
                        e.wait_ge(d.dma_tile.sem, d.dma_count)
                    else:
                        e.wait_ge(sems[d.eng], d.sigcount)
                ins = I.fn(e)
                if ins is None:
                    continue
                if I.is_dma:
                    ins.then_inc(I.dma_tile.sem, 16)
                elif I.signals:
                    ins.then_inc(sems[name], 1)

        with nc.Block() as block:
            @block.tensor
            def _(e):
                run(e, "pe")

            @block.scalar
            def _(e):
                run(e, "act")

            @block.vector
            def _(e):
                run(e, "dve")

            @block.gpsimd
            def _(e):
                run(e, "pool")

            @block.sync
            def _(e):
                run(e, "sp")


class Cfg:
    def __init__(self, nseq=NSEQ, nlayers=NL, stop=None):
        self.nseq = nseq
        self.nlayers = nlayers
        self.stop = stop


def build_program(cfg):
    nc = bass.Bass("TRN2", target_bir_lowering=False)
    P = Prog(nc)
    nseq, nlayers = cfg.nseq, cfg.nlayers

    def din(name, shape, dt=F32):
        return P.tile(name, nc.dram_tensor(name, list(shape), dt, kind="ExternalInput").ap())

    x_d = din("x", [nseq, S, D])
    p_d = din("p", [NL, nseq, S, 256])
    pos_d = din("pos", [nseq, 128, NT], I32)
    w_in_d = din("w_in", [NL, D, 928])
    w_q_d = din("w_q_up", [NL, 256, 768])
    w_kv_d = din("w_kv_up", [NL, 128, 1024])
    w_glu_d = din("w_glu", [NL, 512, 512])
    w_out_d = din("w_out", [NL, D, D])
    w_rt_d = din("w_rt", [NL, 128, KT, 36])
    w_eg_d = din("w_exp_gate", [NL, 32, D, 256])
    w_eu_d = din("w_exp_up", [NL, 32, D, 256])
    w_ed_d = din("w_exp_down", [NL, 32, 256, D])
    w_pg_d = din("w_ple_gate", [NL, D, D])
    w_pp_d = din("w_ple_proj", [NL, 256, D])
    par_d = din("par", [NL, 128, NPAR])
    sA_d = din("sA", [NL, 128, 5, 256])
    sP_d = din("sP", [NL, 128, 3, 32])
    sC_d = din("sC", [NL, 128, 2, 512])
    cst_d = din("cst", [128, 1024])
    gf_d = din("gfin", [128, D])
    out_d = P.tile("out", nc.dram_tensor("out", [nseq, S, D], F32, kind="ExternalOutput").ap())
    dbg = {}

    ARENA = 51200
    arena = nc.alloc_sbuf_tensor("arena", [128, ARENA], F32).ap()
    OFF_H, OFF_XT, OFF_RA, OFF_WB, OFF_MISC = 0, 16384, 24576, 41984, 47104

    def f32(off, n):
        return arena[:, off:off + n]

    def b16(off, n_bf16):
        return arena[:, off:off + (n_bf16 + 1) // 2].bitcast(BF16)

    class Region:
        def __init__(self, base, size):
            self.base, self.size, self.cur = base, size, base
            self.hist = []

        def reset(self, to=None):
            self.cur = self.base if to is None else to

        def register(self, lo, hi, v):
            self.hist.append((lo, hi, v))

        def _mk(self, name, ap, words):
            lo, hi = self.cur, self.cur + words
            assert hi <= self.base + self.size, f"region overflow {name}: {hi - self.base} > {self.size}"
            v = P.tile(name, ap)
            olds = [o for (l, h_, o) in self.hist if l < hi and lo < h_]
            Prog.inherit(v, olds)
            self.hist = [(l, h_, o) for (l, h_, o) in self.hist if not (lo <= l and h_ <= hi)]
            self.hist.append((lo, hi, v))
            self.cur = hi
            return v

        def f32(self, name, n):
            return self._mk(name, f32(self.cur, n), n)

        def b16(self, name, n):
            return self._mk(name, b16(self.cur, n), (n + 1) // 2)

        def reactivate(self, lo, hi, views):
            olds = [o for (l, h_, o) in self.hist if l < hi and lo < h_ and all(o is not v for v in views)]
            for v in views:
                Prog.inherit(v, olds)
            self.hist = [(l, h_, o) for (l, h_, o) in self.hist if not (lo <= l and h_ <= hi)]
            for v in views:
                self.hist.append((lo, hi, v))

    RA = Region(OFF_RA, 17408)
    WB = Region(OFF_WB, 5120)
    XTR = Region(OFF_XT, 8192)
    MS = Region(OFF_MISC, 4096)

    h = [P.tile(f"h{t}", f32(OFF_H + t * D, D)) for t in range(NT)]
    xT_ap = b16(OFF_XT, KT * S).rearrange("p (k t) -> p k t", k=KT)
    xT = [P.tile(f"xT{t}", xT_ap[:, :, t * 128:(t + 1) * 128]) for t in range(NT)]

    def xT_activate():
        XTR.reactivate(OFF_XT, OFF_XT + 8192, xT)

    ident_f = MS.f32("ident_f", 128)
    ident_b = MS.b16("ident_b", 128)
    negmask = MS.b16("negmask", 128)
    cmisc = MS.f32("cmisc", 64)
    iota = MS.f32("iota", 512)
    par = MS.f32("par", NPAR)
    ropeS = MS.f32("ropeS", NT * 16)
    ropeC = MS.f32("ropeC", NT * 16)
    Wt = MS.f32("Wt", NT * 32)
    rstd = MS.f32("rstd", NT)
    rstd_a = MS.f32("rstd_a", NT)
    rstd_s = MS.f32("rstd_s", NT)
    ssqa = MS.f32("ssqa", 2 * NT)
    smallA = [MS.f32(f"smallA{i}", 8) for i in range(2)]
    smallB = [MS.f32(f"smallB{i}", 8) for i in range(2)]
    hn_b = [MS.b16(f"hn_b{i}", D) for i in range(2)]
    ones_b = MS.b16("ones_b", 2)
    th_t = MS.f32("th_t", 32)
    rm_t = MS.f32("rm_t", 32)
    st_t = MS.f32("st_t", 32)
    posf = MS.f32("posf", NT)

    psum = [P.tile(f"ps{i}", nc.alloc_psum_tensor(f"ps{i}", [128, 512], F32).ap()) for i in range(8)]
    for v_ in psum:
        v_.tiles[0].excl = True

    def psb(i):
        return psum[i].cast(BF16)

    rowmask = cmisc[:, 0:8]
    sgn1 = cmisc[:, 8:9]
    Mcol = cmisc[:, 9:10]
    nMcol = cmisc[:, 10:11]
    hpi = cmisc[:, 11:12]
    invf = cmisc[:, 16:32]

    P.dma("sp", ident_f, cst_d[:, 0:128])
    P.dma("pool", ident_b, cst_d[:, 0:128])
    P.dma("pool", negmask, cst_d[:, 128:256])
    P.dma("sp", cmisc, cst_d[:, 256:320])
    P.dma("sp", iota, cst_d[:, 320:832])
    P.memset("dve", ones_b, 1.0)

    def dump(name, v, shape):
        d = P.tile("o_" + name, nc.dram_tensor(name, list(shape), v.ap.dtype, kind="ExternalOutput").ap())
        P.dma("sp", d, v)
        dbg[name] = d
        return d

    def phase_norm(gcol, extra=None):
        g = par[:, gcol:gcol + KT]
        for t in range(NT):
            hb = hn_b[t % 2]
            rs = rstd[:, t:t + 1]
            sq = smallA[t % 2][:, 0:1]
            P.act(hb, h[t], AF.Square, accum=sq)
            P.act(sq, sq, AF.Sqrt, scale=1.0 / D, bias=EPS)
            P.recip(rs, sq)
            P.ts("pool", hb, h[t], rs, None, ALU.mult)
            bank = psb(6 + t % 2)
            for k in range(KT):
                P.tr(bank[:, k * 128:(k + 1) * 128], hb[:, k * 128:(k + 1) * 128], ident_b, acc=(k > 0))
            P.tt("dve", xT[t], bank.re("p (k t) -> p k t", k=KT), g.un(2).bc([128, KT, 128]), ALU.mult)
            if extra is not None:
                extra(t, rs)

    def run_layer(s, l):
        stop = cfg.stop
        if stop == (l, "init"):
            return "init"
        P.dma("sp", par, par_d[l])
        WB.reset()
        RA.reset()
        w_in = WB.b16("w_in", KT * 928).re("p (k n) -> p k n", k=KT)
        P.dma("pool", w_in, w_in_d[l].re("(k p) n -> p k n", p=128))
        wq = WB.b16("wq", 2 * 768).re("p (k n) -> p k n", k=2)
        P.dma("pool", wq, w_q_d[l].re("(k p) n -> p k n", p=128))
        wkv = WB.b16("wkv", 1024)
        P.dma("pool", wkv, w_kv_d[l])
        uT_all = RA.b16("uT", 4 * S).re("p (k t) -> p k t", k=4)
        attnT = RA.b16("attnT", 4 * S).re("p (k t) -> p k t", k=4)
        ra_after_attnT = RA.cur
        Vp = RA.b16("Vp", NT * 8 * 65).re("p (t h d) -> p t h d", t=NT, h=8)
        qnT = RA.b16("qnT", 2 * S).re("p (k t) -> p k t", k=2)
        kvnT = RA.b16("kvnT", S)
        kpe = RA.b16("kpe", NT * 32).re("p (t d) -> p t d", t=NT)
        PT = [RA.b16(f"PT{i}", 512) for i in range(4)]

        xT_activate()
        phase_norm(0)
        if stop == (l, "norm0"):
            return "norm0"
        if stop is not None and stop[1].startswith("cut"):
            P.budget = int(stop[1][3:])
        if stop == (l, "norm"):
            for t in range(NT):
                dump(f"xT_{t}", xT[t], [128, KT, 128])
            return "norm"
        for t in range(NT):
            tc = slice(t * 128, (t + 1) * 128)
            za, zb = psum[0 + 2 * (t % 2)], psum[1 + 2 * (t % 2)]
            for k in range(KT):
                P.mm(za[:, 0:416], xT[t][:, k, :], w_in[:, k, 0:416], start=(k == 0), stop=(k == KT - 1))
            for k in range(KT):
                P.mm(zb[:, 0:512], xT[t][:, k, :], w_in[:, k, 416:928], start=(k == 0), stop=(k == KT - 1))
            sa = smallB[t % 2]
            hb = hn_b[t % 2]
            P.act(hb[:, 0:256], za[:, 0:256], AF.Square, accum=sa[:, 1:2])
            P.act(hb[:, 256:384], za[:, 256:384], AF.Square, accum=sa[:, 2:3])
            P.act(sa[:, 1:2], sa[:, 1:2], AF.Sqrt, scale=1.0 / 256, bias=EPS)
            P.act(sa[:, 2:3], sa[:, 2:3], AF.Sqrt, scale=1.0 / 128, bias=EPS)
            P.recip(sa[:, 3:5], sa[:, 1:3])
            P.ts("dve", hb[:, 0:256], za[:, 0:256], sa[:, 3:4], None, ALU.mult)
            P.ts("dve", hb[:, 256:384], za[:, 256:384], sa[:, 4:5], None, ALU.mult)
            x1, x2 = za[:, 384:400], za[:, 400:416]
            cs, sn = ropeC[:, t * 16:(t + 1) * 16], ropeS[:, t * 16:(t + 1) * 16]
            tmp = hb[:, 896:1024].cast(F32)
            P.tt("dve", tmp[:, 0:16], x1, cs, ALU.mult)
            P.tt("dve", tmp[:, 16:32], x2, sn, ALU.mult)
            P.tt("dve", tmp[:, 32:48], x1, sn, ALU.mult)
            P.tt("dve", tmp[:, 48:64], x2, cs, ALU.mult)
            P.tt("pool", kpe[:, t, 0:16], tmp[:, 0:16], tmp[:, 16:32], ALU.subtract)
            P.tt("pool", kpe[:, t, 16:32], tmp[:, 32:48], tmp[:, 48:64], ALU.add)
            P.copy("act", hb[:, 384:896], zb[:, 0:512])
            bank = psb(4 + t % 2)
            for j in range(7):
                P.tr(bank[:, j * 128:(j + 1) * 128], hb[:, j * 128:(j + 1) * 128], ident_b, acc=(j > 0))
            b3 = bank.re("p (k t) -> p k t", k=8)
            P.tt("dve", qnT[:, :, tc], b3[:, 0:2, :], par[:, 24:26].un(2).bc([128, 2, 128]), ALU.mult)
            P.ts("dve", kvnT[:, tc], bank[:, 256:384], par[:, 26:27], None, ALU.mult)
            P.copy("dve", uT_all[:, :, tc], b3[:, 3:7, :])
        if stop == (l, "A1"):
            dump("uT", uT_all.re("p k t -> p (k t)"), [128, 4 * S])
            dump("qnT", qnT.re("p k t -> p (k t)"), [128, 2 * S])
            dump("kvnT", kvnT, [128, S])
            dump("kpe", kpe.re("p t d -> p (t d)"), [128, NT * 32])
            return "A1"

        XTR.reset()
        qTh = XTR.b16("qTh", 4 * S).re("p (h t) -> p h t", h=4)
        kTh = XTR.b16("kTh", 4 * S).re("p (h t) -> p h t", h=4)
        P.memset("pool", Vp.re("p t h d -> p (t h) d")[:, :, 64:65], 1.0)
        for hh in range(2):
            for t in range(NT):
                tc = slice(t * 128, (t + 1) * 128)
                qps, kvps = psum[0 + 2 * (t % 2)], psum[1 + 2 * (t % 2)]
                for k in range(2):
                    P.mm(qps[:, 0:384], qnT[:, k, tc], wq[:, k, hh * 384:(hh + 1) * 384], start=(k == 0), stop=(k == 1))
                P.mm(kvps[:, 0:512], kvnT[:, tc], wkv[:, hh * 512:(hh + 1) * 512], start=True, stop=True)
                hb = hn_b[t % 2]
                q_b = hb[:, 0:384].re("p (h d) -> p h d", h=4)
                k_b = hb[:, 384:768].re("p (h d) -> p h d", h=4)
                tmp = hb[:, 768:1024].cast(F32).re("p (a h d) -> p a h d", a=2, h=4)
                q3 = qps[:, 0:384].re("p (h d) -> p h d", h=4)
                kv3 = kvps[:, 0:512].re("p (h d) -> p h d", h=4)
                cs = ropeC[:, t * 16:(t + 1) * 16].un(1).bc([128, 4, 16])
                sn = ropeS[:, t * 16:(t + 1) * 16].un(1).bc([128, 4, 16])
                P.copy("act", q_b[:, :, 0:64], q3[:, :, 0:64])
                P.tt("dve", tmp[:, 0], q3[:, :, 64:80], cs, ALU.mult)
                P.tt("dve", tmp[:, 1], q3[:, :, 80:96], sn, ALU.mult)
                P.tt("pool", q_b[:, :, 64:80], tmp[:, 0], tmp[:, 1], ALU.subtract)
                P.tt("dve", tmp[:, 0], q3[:, :, 64:80], sn, ALU.mult)
                P.tt("dve", tmp[:, 1], q3[:, :, 80:96], cs, ALU.mult)
                P.tt("pool", q_b[:, :, 80:96], tmp[:, 0], tmp[:, 1], ALU.add)
                P.copy("act", k_b[:, :, 0:64], kv3[:, :, 0:64])
                P.copy("pool", k_b[:, :, 64:96], kpe[:, t, :].un(1).bc([128, 4, 32]))
                P.copy("act", Vp[:, t, hh * 4:(hh + 1) * 4, 0:64], kv3[:, :, 64:128])
                bank = psb(4 + t % 2)
                for j in range(4):
                    P.tr(bank[0:96, j * 128:(j + 1) * 128], q_b[:, j, :], ident_b, acc=(j > 0))
                for j in range(4):
                    P.tr(bank[0:96, (4 + j) * 128:(5 + j) * 128], k_b[:, j, :], ident_b, acc=True)
                b3 = bank.re("p (k t) -> p k t", k=8)
                P.copy("dve", qTh[0:96, :, tc], b3[0:96, 0:4, :])
                P.copy("dve", kTh[0:96, :, tc], b3[0:96, 4:8, :])
            if stop == (l, "A2") and hh == 0:
                dump("qTh", qTh[0:96].re("p h t -> p (h t)"), [96, 4 * S])
                dump("kTh", kTh[0:96].re("p h t -> p (h t)"), [96, 4 * S])
                dump("Vp", Vp.re("p t h d -> p (t h d)"), [128, NT * 8 * 65])
                return "A2"
            sbank = [psum[0], psum[1], psum[2]]
            abank = [psum[3], psum[7]]
            si = 0
            pi = 0
            for c in range(4):
                ach = hn_b[c % 2]
                a4 = ach[:, 0:1024].re("p (q d) -> p q d", q=4)
                for j in range(4):
                    hd = hh * 4 + j
                    acc = abank[(c * 4 + j) % 2][:, 0:260].re("p (q d) -> p q d", q=4)
                    nk = 4 * c + 4
                    for kt in range(nk):
                        r = kt - 4 * c
                        sb = sbank[si % 3]
                        si += 1
                        kc = slice(kt * 128, (kt + 1) * 128)
                        if r < 0:
                            P.mm(sb[:, 0:512], kTh[0:96, j, kc], qTh[0:96, j, c * 512:(c + 1) * 512])
                            lo = 0
                        else:
                            lo = r * 128
                            P.mm(sb[:, lo:lo + 128], ident_b, negmask, start=True, stop=False)
                            P.mm(sb[:, lo:lo + 128], kTh[0:96, j, kc], qTh[0:96, j, c * 512 + lo:c * 512 + lo + 128], start=False, stop=True)
                            if lo + 128 < 512:
                                P.mm(sb[:, lo + 128:512], kTh[0:96, j, kc], qTh[0:96, j, c * 512 + lo + 128:(c + 1) * 512])
                        pt = PT[pi % 4]
                        pi += 1
                        P.act(pt[:, lo:512], sb[:, lo:512], AF.Exp, scale=ATTN_SCALE)
                        for qi in range(max(0, r), 4):
                            P.mm(acc[:, qi, :], pt[:, qi * 128:(qi + 1) * 128], Vp[:, kt, hd, :],
                                 start=(kt == 0 and qi == 0), stop=(kt == 4 * c + qi))
                    rd = smallA[j % 2][:, 4:8]
                    P.recip(rd, acc[:, :, 64])
                    P.tt("dve", a4[:, :, j * 64:(j + 1) * 64], acc[:, :, 0:64], rd.un(2).bc([128, 4, 64]), ALU.mult)
                for qi in range(4):
                    t = 4 * c + qi
                    tc = slice(t * 128, (t + 1) * 128)
                    junk = PT[qi][:, 0:256]
                    P.act(junk, a4[:, qi, :], AF.Square, accum=ssqa[:, hh * NT + t:hh * NT + t + 1])
                    bank = psb(5 + qi % 2)
                    for m in range(2):
                        P.tr(bank[:, m * 128:(m + 1) * 128], a4[:, qi, m * 128:(m + 1) * 128], ident_b, acc=(m > 0))
                    P.tt("dve", attnT[:, 2 * hh:2 * hh + 2, tc], bank[:, 0:256].re("p (k t) -> p k t", k=2),
                         par[:, 28 + 2 * hh:30 + 2 * hh].un(2).bc([128, 2, 128]), ALU.mult)
        P.tt("dve", rstd_a, ssqa[:, 0:NT], ssqa[:, NT:2 * NT], ALU.add)
        P.act(rstd_a, rstd_a, AF.Sqrt, scale=1.0 / 512, bias=EPS)
        P.recip(rstd_a, rstd_a)
        if stop == (l, "B"):
            dump("attnT", attnT.re("p k t -> p (k t)"), [128, 4 * S])
            dump("rstd_a", rstd_a, [128, NT])
            return "B"

        RA.reset(ra_after_attnT)
        ssmT = RA.b16("ssmT", 4 * S).re("p (k t) -> p k t", k=4)
        BP = RA.f32("BP", 512).re("p (o n) -> p o n", o=4)
        BPS = RA.f32("BPS", 512).re("p (o n) -> p o n", o=4)
        Craw = RA.b16("Craw", 2 * 512).re("p (a g c) -> p a g c", a=2, g=32)
        Bpad = [[RA.b16(f"Bpad{a}_{g}", 128) for g in range(8)] for a in range(2)]
        Cpad = [[RA.b16(f"Cpad{a}_{g}", 128) for g in range(8)] for a in range(2)]
        wglu = RA.b16("wglu", 4 * 512).re("p (k n) -> p k n", k=4)
        P.dma("pool", wglu, w_glu_d[l].re("(k p) n -> p k n", p=128))
        XTR.reset()
        sA = XTR.f32("sA", 5 * 256).re("p (a n) -> p a n", a=5)
        P.dma("sp", sA, sA_d[l])
        T_ = [XTR.f32(f"s5t{i}", 256) for i in range(7)]
        sPt = XTR.f32("sPt", 96).re("p (a g) -> p a g", a=3)
        P.dma("sp", sPt, sP_d[l])
        T2 = [XTR.f32(f"s5u{i}", 32) for i in range(2)]
        sCf = XTR.f32("sCf", 1024).re("p (a g c) -> p a g c", a=2, g=32)
        P.dma("sp", sCf, sC_d[l].re("p a (g c) -> p a g c", g=32))
        P.ts("dve", Craw[:, 0], sCf[:, 0], sgn1, None, ALU.mult)
        P.ts("dve", Craw[:, 1], sCf[:, 1], -1.0, None, ALU.mult)
        for a in range(2):
            for g in range(8):
                P.memset("pool", Cpad[a][g], 0.0)

        def lam_terms(are, aim, ls, tmp, out_er, out_f):
            dt_, ph = tmp
            P.act(dt_, ls, AF.Exp)
            P.tt("dve", out_er, are, dt_, ALU.mult)
            P.act(out_er, out_er, AF.Exp)
            P.tt("dve", ph, aim, dt_, ALU.mult)
            P.ts("dve", ph, ph, 1.0 / TWO_PI, None, ALU.mult)
            P.act(dt_, ph, AF.Identity, bias=Mcol)
            P.act(dt_, dt_, AF.Identity, bias=nMcol)
            P.tt("dve", out_f, ph, dt_, ALU.subtract)

        lam_terms(sPt[:, 0], sPt[:, 1], sPt[:, 2], (T2[0], T2[1]), rm_t, th_t)
        er, fr = T_[0], T_[1]
        lam_terms(sA[:, 0], sA[:, 1], sA[:, 2], (T_[2], T_[3]), er, fr)
        sn_, cs_ = T_[2], T_[3]
        P.act(sn_, fr, AF.Sin, scale=TWO_PI)
        P.act(fr, fr, AF.Abs)
        P.act(cs_, fr, AF.Sin, scale=-TWO_PI, bias=hpi)
        lre, lim = T_[4], T_[5]
        P.tt("dve", lre, er, cs_, ALU.mult)
        P.tt("dve", lim, er, sn_, ALU.mult)
        P.ts("dve", lre, lre, -1.0, None, ALU.add)
        den = T_[0]
        P.tt("dve", den, sA[:, 0], sA[:, 0], ALU.mult)
        P.tt("dve", T_[1], sA[:, 1], sA[:, 1], ALU.mult)
        P.tt("dve", den, den, T_[1], ALU.add)
        P.recip(den, den)
        fre, fim = T_[2], T_[3]
        P.tt("dve", fre, lre, sA[:, 0], ALU.mult)
        P.tt("dve", T_[1], lim, sA[:, 1], ALU.mult)
        P.tt("dve", fre, fre, T_[1], ALU.add)
        P.tt("dve", fre, fre, den, ALU.mult)
        P.tt("dve", fim, lim, sA[:, 0], ALU.mult)
        P.tt("dve", T_[1], lre, sA[:, 1], ALU.mult)
        P.tt("dve", fim, fim, T_[1], ALU.subtract)
        P.tt("dve", fim, fim, den, ALU.mult)
        bre, bim = sA[:, 3], sA[:, 4]

        def o4(v):
            return v.re("p (o n) -> p o n", o=4)
        P.tt("dve", T_[4], fre, bre, ALU.mult)
        P.tt("dve", T_[5], fim, bim, ALU.mult)
        P.tt("dve", BP[:, :, 0:64], o4(T_[4]), o4(T_[5]), ALU.subtract)
        P.tt("dve", T_[4], fre, bim, ALU.mult)
        P.tt("dve", T_[5], fim, bre, ALU.mult)
        P.tt("dve", BP[:, :, 64:128], o4(T_[4]), o4(T_[5]), ALU.add)
        P.copy("dve", BPS[:, :, 0:64], BP[:, :, 64:128])
        P.ts("dve", BPS[:, :, 64:128], BP[:, :, 0:64], -1.0, None, ALU.mult)
        P.memset("dve", st_t, 0.0)
        if stop == (l, "C0"):
            dump("BP", BP.re("p o n -> p (o n)"), [128, 512])
            dump("th_t", th_t, [128, 32])
            dump("rm_t", rm_t, [128, 32])
            dump("Craw", Craw.re("p a g c -> p (a g c)"), [128, 1024])
            return "C0"

        XTR.reset()
        Stab = [XTR.f32(f"Stab{i}", 512) for i in range(2)]
        Ctab = [XTR.f32(f"Ctab{i}", 512) for i in range(2)]
        ftmp = [XTR.f32(f"ftmp{i}", 512) for i in range(2)]
        t1b = [XTR.f32(f"t1b{i}", 512) for i in range(2)]
        t2b = [XTR.f32("t2b0", 512)]
        sst = [XTR.f32(f"sst{i}", 512) for i in range(2)]
        P1 = [XTR.b16(f"P1_{i}", 512) for i in range(2)]
        P2 = [XTR.b16(f"P2_{i}", 512) for i in range(2)]
        yv = [XTR.f32("yv0", 512)]
        gt = [XTR.f32(f"gt{i}", 512) for i in range(2)]
        it = 0
        for o in range(4):
            for gl in range(8):
                g = o * 8 + gl
                P.ts("pool", Bpad[0][gl], BP[:, o, :], rowmask[:, gl:gl + 1], None, ALU.mult)
                P.ts("pool", Bpad[1][gl], BPS[:, o, :], rowmask[:, gl:gl + 1], None, ALU.mult)
                P.copy("pool", Cpad[0][gl][:, gl * 16:(gl + 1) * 16], Craw[:, 0, g, :])
                P.copy("pool", Cpad[1][gl][:, gl * 16:(gl + 1) * 16], Craw[:, 1, g, :])
            for b in range(4):
                bc_ = slice(b * 512, (b + 1) * 512)
                yps = psum[4 + (o * 4 + b) % 2]
                for gl in range(8):
                    g = o * 8 + gl
                    i2 = it % 2
                    it += 1
                    th = th_t[:, g:g + 1]
                    f_ = ftmp[i2]
                    P.ts("pool", f_, iota, float(b * 512), th, ALU.add, ALU.mult)
                    P.act(Stab[i2], f_, AF.Identity, bias=Mcol)
                    P.act(Stab[i2], Stab[i2], AF.Identity, bias=nMcol)
                    P.tt("pool", f_, f_, Stab[i2], ALU.subtract)
                    P.act(Stab[i2], f_, AF.Sin, scale=TWO_PI)
                    P.act(f_, f_, AF.Abs)
                    P.act(Ctab[i2], f_, AF.Sin, scale=-TWO_PI, bias=hpi)
                    pa, pb_ = psum[0 + 2 * i2], psum[1 + 2 * i2]
                    P.mm(pa, Bpad[0][gl], uT_all[:, o, bc_])
                    P.mm(pb_, Bpad[1][gl], uT_all[:, o, bc_])
                    P.tt("dve", t1b[i2], pa, Ctab[i2], ALU.mult)
                    P.tt("dve", t2b[0], pb_, Stab[i2], ALU.mult)
                    P.tt("pool", t1b[i2], t1b[i2], t2b[0], ALU.add)
                    P.scan(sst[i2], rm_t[:, g:g + 1].bc([128, 512]), t1b[i2], st_t[:, g:g + 1] if b > 0 else 0.0)
                    if b < 3:
                        P.copy("pool", st_t[:, g:g + 1], sst[i2][:, 511:512])
                    P.tt("dve", P1[i2], sst[i2], Ctab[i2], ALU.mult)
                    P.tt("pool", P2[i2], sst[i2], Stab[i2], ALU.mult)
                    P.mm(yps, Cpad[0][gl], P1[i2], start=(gl == 0), stop=False)
                    P.mm(yps, Cpad[1][gl], P2[i2], start=False, stop=(gl == 7))
                y_ = yv[0]
                g_ = gt[(o * 4 + b) % 2]
                P.stt("dve", y_, uT_all[:, o, bc_], par[:, 40 + o:41 + o], yps, ALU.mult, ALU.add)
                P.act(g_, y_, AF.Square)
                P.ts("pool", g_, g_, 0.044715, 1.0, ALU.mult, ALU.add)
                P.tt("pool", g_, g_, y_, ALU.mult)
                P.act(g_, g_, AF.Sigmoid, scale=1.5957691216)
                P.tt("pool", ssmT[:, o, bc_], y_, g_, ALU.mult)
        if stop == (l, "C1"):
            dump("yg", ssmT.re("p k t -> p (k t)"), [128, 4 * S])
            return "C1"
        XTR.reset()
        sq4 = [XTR.b16(f"sq4_{m}", 512) for m in range(4)]
        gg = [XTR.f32(f"gg{i}", 512) for i in range(2)]
        for b in range(4):
            bc_ = slice(b * 512, (b + 1) * 512)
            for m in range(4):
                gp = psum[m]
                for k in range(4):
                    P.mm(gp, wglu[:, k, m * 128:(m + 1) * 128], ssmT[:, k, bc_], start=(k == 0), stop=(k == 3))
            if stop == (l, "C2") and b == 0:
                dbuf = XTR.f32("dbuf", 2048)
                for m in range(4):
                    P.copy("dve", dbuf[:, m * 512:(m + 1) * 512], psum[m])
                dump("glupre", dbuf, [128, 2048])
                dump("wglu", wglu.re("p k n -> p (k n)"), [128, 2048])
                return "C2"
            for m in range(4):
                sg = gg[m % 2]
                P.act(sg, psum[m], AF.Sigmoid, bias=par[:, 36 + m:37 + m])
                P.tt("pool", sg, sg, ssmT[:, m, bc_], ALU.mult)
                P.tt("pool", sq4[m], sg, sg, ALU.mult)
                P.ts("dve", ssmT[:, m, bc_], sg, par[:, 32 + m:33 + m], None, ALU.mult)
            for q in range(4):
                t = b * 4 + q
                sp_ = psum[4 + q % 2]
                for m in range(4):
                    P.mm(sp_[:, 0:1], sq4[m][:, q * 128:(q + 1) * 128], ones_b[:, 0:1], start=(m == 0), stop=(m == 3))
                P.copy("dve", rstd_s[:, t:t + 1], sp_[:, 0:1])
        P.act(rstd_s, rstd_s, AF.Sqrt, scale=1.0 / 512, bias=EPS)
        P.recip(rstd_s, rstd_s)
        if stop == (l, "C"):
            dump("ssmT", ssmT.re("p k t -> p (k t)"), [128, 4 * S])
            dump("rstd_s", rstd_s, [128, NT])
            return "C"

        WB.reset()
        wout = WB.b16("wout", KT * D).re("p (k n) -> p k n", k=KT)
        P.dma("pool", wout, w_out_d[l].re("(k p) n -> p k n", p=128))
        for t in range(NT):
            tc = slice(t * 128, (t + 1) * 128)
            for src, koff, rs, pb0 in ((attnT, 0, rstd_a, 0), (ssmT, 4, rstd_s, 2)):
                for nh in range(2):
                    pp = psum[pb0 + nh + 4 * (t % 2)]
                    for k in range(4):
                        P.mm(pp, src[:, k, tc], wout[:, koff + k, nh * 512:(nh + 1) * 512], start=(k == 0), stop=(k == 3))
                    hv = h[t][:, nh * 512:(nh + 1) * 512]
                    P.stt("dve", hv, pp, rs[:, t:t + 1], hv, ALU.mult, ALU.add)
        if stop == (l, "D"):
            return "D"

        RA.reset()
        wexp = []
        for i in range(2):
            wg = RA.b16(f"wg{i}", KT * 256).re("p (k n) -> p k n", k=KT)
            wu = RA.b16(f"wu{i}", KT * 256).re("p (k n) -> p k n", k=KT)
            wd = RA.b16(f"wd{i}", 2 * D).re("p (k n) -> p k n", k=2)
            wexp.append((wg, wu, wd))
        hdn = [[RA.b16(f"hdn{i}_{m}", 512) for m in range(2)] for i in range(2)]
        sgt = [RA.f32(f"sgt{i}", 512) for i in range(4)]
        hn32 = RA.f32("hn32", D)
        hnT32 = RA.f32("hnT32", D).re("p (k t) -> p k t", k=KT)
        wrt = RA.f32("wrt", KT * 36).re("p (k n) -> p k n", k=KT)
        P.dma("sp", wrt, w_rt_d[l])
        rt = [RA.f32(f"rt{i}", 128) for i in range(2)]

        def load_expert(e):
            wg, wu, wd = wexp[e % 2]
            P.dma("pool", wg, w_eg_d[l, e].re("(k p) n -> p k n", p=128))
            P.dma("pool", wu, w_eu_d[l, e].re("(k p) n -> p k n", p=128))
            P.dma("pool", wd, w_ed_d[l, e].re("(k p) n -> p k n", p=128))

        load_expert(0)
        load_expert(1)
        gm = par[:, 8:16]

        def router(t, rs):
            P.ts("dve", hn32, h[t], rs, None, ALU.mult)
            b0, b1 = psum[2], psum[3]
            for k in range(KT):
                bk = b0 if k < 4 else b1
                P.tr(bk[:, (k % 4) * 128:(k % 4 + 1) * 128], hn32[:, k * 128:(k + 1) * 128], ident_f, acc=(k % 4 > 0))
            P.tt("dve", hnT32[:, 0:4, :], b0.re("p (k t) -> p k t", k=4), gm[:, 0:4].un(2).bc([128, 4, 128]), ALU.mult)
            P.tt("dve", hnT32[:, 4:8, :], b1.re("p (k t) -> p k t", k=4), gm[:, 4:8].un(2).bc([128, 4, 128]), ALU.mult)
            lp = psum[4 + t % 2]
            for k in range(KT):
                P.mm(lp[:, 0:36], hnT32[:, k, :], wrt[:, k, :], start=(k == 0), stop=(k == KT - 1))
            r_ = rt[t % 2]
            lg = r_[:, 0:4]
            le = r_[:, 4:36]
            P.tt("dve", r_[:, 0:36], lp[:, 0:36], par[:, 48:84], ALU.add)
            gmax = r_[:, 36:37]
            P.reduce("dve", gmax, lg, ALU.max)
            ohg = r_[:, 40:44]
            P.ts("dve", ohg, lg, gmax, None, ALU.is_equal)
            ngmax = r_[:, 37:38]
            P.ts("dve", ngmax, gmax, -1.0, None, ALU.mult)
            gsum = r_[:, 38:39]
            P.act(r_[:, 44:48], lg, AF.Exp, bias=ngmax, accum=gsum)
            tmp = r_[:, 48:80]
            P.tt("dve", tmp.re("p (g j) -> p g j", g=4), le.re("p (g j) -> p g j", g=4), ohg.un(2).bc([128, 4, 8]), ALU.mult)
            esel = r_[:, 80:88]
            P.reduce("dve", esel, tmp.re("p (g j) -> p j g", g=4), ALU.add)
            m1 = r_[:, 88:89]
            P.reduce("dve", m1, esel, ALU.max)
            oh1 = r_[:, 96:104]
            P.ts("dve", oh1, esel, m1, None, ALU.is_equal)
            es2 = r_[:, 104:112]
            P.stt("dve", es2, oh1, -1e30, esel, ALU.mult, ALU.add)
            m2 = r_[:, 89:90]
            P.reduce("dve", m2, es2, ALU.max)
            oh2 = r_[:, 112:120]
            P.ts("dve", oh2, es2, m2, None, ALU.is_equal)
            dd = r_[:, 90:91]
            P.tt("dve", dd, m2, m1, ALU.subtract)
            ed = r_[:, 91:92]
            P.act(ed, dd, AF.Exp)
            den_ = r_[:, 92:93]
            P.ts("dve", den_, ed, 1.0, None, ALU.add)
            P.tt("dve", den_, den_, gsum, ALU.mult)
            w1 = r_[:, 93:94]
            P.recip(w1, den_)
            w2 = r_[:, 94:95]
            P.tt("dve", w2, w1, ed, ALU.mult)
            inner = r_[:, 120:128]
            P.ts("dve", inner, oh1, w1, None, ALU.mult)
            P.stt("dve", inner, oh2, w2, inner, ALU.mult, ALU.add)
            wt3 = Wt[:, t * 32:(t + 1) * 32].re("p (g j) -> p g j", g=4)
            P.tt("dve", wt3, ohg.un(2).bc([128, 4, 8]), inner.un(1).bc([128, 4, 8]), ALU.mult)

        xT_activate()
        phase_norm(8, extra=router)
        if stop == (l, "E0"):
            dump("Wt", Wt, [128, NT * 32])
            return "E0"
        yi = 0
        for e in range(32):
            wg, wu, wd = wexp[e % 2]
            for b in range(4):
                bc_ = slice(b * 512, (b + 1) * 512)
                xtiles = tuple(xT[4 * b + q].tiles[0] for q in range(4))
                hd_ = hdn[(e * 4 + b) % 2]
                for m in range(2):
                    gp, up = psum[0 + 2 * m], psum[1 + 2 * m]
                    for k in range(KT):
                        P.mm(gp, wg[:, k, m * 128:(m + 1) * 128], V(xtiles, xT_ap[:, k, bc_]), start=(k == 0), stop=(k == KT - 1))
                    for k in range(KT):
                        P.mm(up, wu[:, k, m * 128:(m + 1) * 128], V(xtiles, xT_ap[:, k, bc_]), start=(k == 0), stop=(k == KT - 1))
                    sg = sgt[(b * 2 + m) % 4]
                    P.act(sg, gp, AF.Silu)
                    P.tt("dve", hd_[m], sg, up, ALU.mult)
                for q in range(4):
                    t = 4 * b + q
                    for nh in range(2):
                        yp = psum[4 + yi % 4]
                        yi += 1
                        for m in range(2):
                            P.mm(yp, hd_[m][:, q * 128:(q + 1) * 128], wd[:, m, nh * 512:(nh + 1) * 512], start=(m == 0), stop=(m == 1))
                        hv = h[t][:, nh * 512:(nh + 1) * 512]
                        P.stt("dve", hv, yp, Wt[:, t * 32 + e:t * 32 + e + 1], hv, ALU.mult, ALU.add)
            if e + 2 < 32:
                load_expert(e + 2)
        if stop == (l, "E"):
            return "E"

        RA.reset()
        WB.reset()
        wpg = WB.b16("wpg", KT * D).re("p (k n) -> p k n", k=KT)
        P.dma("pool", wpg, w_pg_d[l].re("(k p) n -> p k n", p=128))
        wpp = RA.b16("wpp", 2 * D).re("p (k n) -> p k n", k=2)
        P.dma("pool", wpp, w_pp_d[l].re("(k p) n -> p k n", p=128))
        pT = RA.b16("pT", 2 * S).re("p (k t) -> p k t", k=2)
        p_b = [RA.b16(f"p_b{i}", 256) for i in range(2)]
        sgp = [RA.f32(f"sgp{i}", 512) for i in range(4)]
        for t in range(NT):
            tc = slice(t * 128, (t + 1) * 128)
            pb_ = p_b[t % 2]
            P.dma("pool", pb_, p_d[l, s, tc, :])
            bank = psb(4 + t % 2)
            for k in range(2):
                P.tr(bank[:, k * 128:(k + 1) * 128], pb_[:, k * 128:(k + 1) * 128], ident_b, acc=(k > 0))
            P.copy("dve", pT[:, :, tc], bank[:, 0:256].re("p (k t) -> p k t", k=2))
        phase_norm(16)
        si = 0
        for t in range(NT):
            tc = slice(t * 128, (t + 1) * 128)
            for nh in range(2):
                gp, pp = psum[0 + 2 * (si % 2)], psum[1 + 2 * (si % 2)]
                for k in range(KT):
                    P.mm(gp, xT[t][:, k, :], wpg[:, k, nh * 512:(nh + 1) * 512], start=(k == 0), stop=(k == KT - 1))
                for k in range(2):
                    P.mm(pp, pT[:, k, tc], wpp[:, k, nh * 512:(nh + 1) * 512], start=(k == 0), stop=(k == 1))
                sg = sgp[si % 4]
                si += 1
                P.act(sg, gp, AF.Sigmoid)
                P.tt("dve", sg, sg, pp, ALU.mult)
                hv = h[t][:, nh * 512:(nh + 1) * 512]
                P.tt("pool", hv, hv, sg, ALU.add)
        if stop == (l, "F"):
            return "F"
        return None

    stopped = None
    for s in range(nseq):
        RA.reset()
        posi = RA.f32("posi", NT).cast(I32)
        P.dma("sp", posi, pos_d[s])
        P.copy("dve", posf, posi)
        tr_ = RA.f32("ropetmp", NT * 16)
        tr2 = RA.f32("ropetmp2", NT * 16)
        t3 = tr_.re("p (t i) -> p t i", t=NT)
        P.tt("dve", t3, posf.un(2).bc([128, NT, 16]), invf.un(1).bc([128, NT, 16]), ALU.mult)
        P.act(tr2, tr_, AF.Identity, bias=Mcol)
        P.act(tr2, tr2, AF.Identity, bias=nMcol)
        P.tt("dve", tr_, tr_, tr2, ALU.subtract)
        P.act(ropeS, tr_, AF.Sin, scale=TWO_PI)
        P.act(tr_, tr_, AF.Abs)
        P.act(ropeC, tr_, AF.Sin, scale=-TWO_PI, bias=hpi)
        for t in range(NT):
            P.dma("sp", h[t], x_d[s, t * 128:(t + 1) * 128, :])
        for l in range(nlayers):
            try:
                stopped = run_layer(s, l)
            except Cut:
                stopped = "cut"
                P.budget = None
            if stopped:
                break
        if stopped:
            for t in range(NT):
                P.dma("sp", out_d[s, t * 128:(t + 1) * 128, :], h[t], sem_tile=out_d)
            break
        RA.reset()
        gf = RA.f32("gf", D)
        P.dma("sp", gf, gf_d)
        ob = [RA.f32(f"ob{i}", D) for i in range(2)]
        for t in range(NT):
            sq = smallA[t % 2][:, 0:1]
            o_ = ob[t % 2]
            P.act(o_, h[t], AF.Square, accum=sq)
            P.act(sq, sq, AF.Sqrt, scale=1.0 / D, bias=EPS)
            P.recip(sq, sq)
            P.stt("dve", o_, h[t], sq, gf, ALU.mult, ALU.mult)
            P.dma("sp", out_d[s, t * 128:(t + 1) * 128, :], o_, sem_tile=out_d)

    fin = [out_d] + list(dbg.values())
    P.add("sp", lambda e: None, reads=fin, writes=())
    P.emit()
    return nc, list(dbg.keys())


def _consts():
    c = np.zeros((128, 1024), np.float32)
    c[:, 0:128] = np.eye(128, dtype=np.float32)
    pp, jj = np.meshgrid(np.arange(128), np.arange(128), indexing="ij")
    c[:, 128:256] = np.where(pp > jj, -30000.0, 0.0)
    cm = np.zeros((128, 64), np.float32)
    for gl in range(8):
        cm[gl * 16:(gl + 1) * 16, gl] = 1.0
    cm[:64, 8] = 1.0
    cm[64:, 8] = -1.0
    cm[:, 9] = MAGIC
    cm[:, 10] = -MAGIC
    cm[:, 11] = math.pi / 2
    cm[:, 12] = -1.0
    half = 16
    inv_freq = 10000.0 ** (-np.arange(half, dtype=np.float32) / half)
    cm[:, 16:32] = (inv_freq.astype(np.float64) / TWO_PI).astype(np.float32)[None, :]
    c[:, 256:320] = cm
    c[:, 320:832] = np.arange(512, dtype=np.float32)[None, :]
    return c


def _fm(v, n):
    return np.ascontiguousarray(v.reshape(NL, n // 128, 128).transpose(0, 2, 1))


def host_layout(inputs):
    f = lambda k: np.asarray(inputs[k], dtype=np.float32)
    par = np.zeros((NL, 128, NPAR), np.float32)
    par[:, :, 0:8] = _fm(f("g_mix_norm"), 1024)
    par[:, :, 8:16] = _fm(f("g_moe_norm"), 1024)
    par[:, :, 16:24] = _fm(f("g_ple_norm"), 1024)
    par[:, :, 24:26] = _fm(f("g_q_lat"), 256)
    par[:, :, 26:27] = _fm(f("g_kv_lat"), 128)
    par[:, :, 28:32] = _fm(f("g_attn_out"), 512)
    par[:, :, 32:36] = _fm(f("g_ssm_out"), 512)
    par[:, :, 36:40] = _fm(f("b_glu"), 512)
    par[:, :, 40:44] = _fm(f("ssm_d").reshape(NL, 512), 512)
    par[:, :, 48:52] = f("b_group_router")[:, None, :]
    par[:, :, 52:84] = f("b_expert_router")[:, None, :]
    a_re, a_im, ls = f("ssm_a_re"), f("ssm_a_im"), f("ssm_log_step")
    b_re, b_im = f("ssm_b_re"), f("ssm_b_im")
    c_re, c_im = f("ssm_c_re"), f("ssm_c_im")

    def l1(a):
        a = a.reshape(NL, 4, 8, 64).transpose(0, 2, 1, 3)
        a = np.broadcast_to(a[:, :, None, :, :], (NL, 8, 16, 4, 64))
        return a.reshape(NL, 128, 256)

    def l1b(b):
        b = b.reshape(NL, 4, 8, 64, 16).transpose(0, 2, 4, 1, 3)
        return b.reshape(NL, 128, 256)

    lsx = np.broadcast_to(ls[:, :, None], (NL, 32, 64))
    sA = np.stack([l1(a_re), l1(a_im), l1(lsx), l1b(b_re), l1b(b_im)], axis=2)

    def l2(a):
        a = a.transpose(0, 2, 1)
        return np.concatenate([a, a], axis=1)

    sP = np.stack([l2(a_re), l2(a_im), l2(lsx)], axis=2)
    cre_t = c_re.transpose(0, 3, 1, 2).reshape(NL, 64, 512)
    cim_t = c_im.transpose(0, 3, 1, 2).reshape(NL, 64, 512)
    sC = np.stack([np.concatenate([cre_t, cim_t], axis=1), np.concatenate([cim_t, cre_t], axis=1)], axis=2)
    w_rt = np.concatenate([f("w_group_router"), f("w_expert_router")], axis=-1)
    w_rt = w_rt.reshape(NL, KT, 128, 36).transpose(0, 2, 1, 3)
    shared = {
        "w_in": f("w_in"), "w_q_up": f("w_q_up"), "w_kv_up": f("w_kv_up"), "w_glu": f("w_glu"), "w_out": f("w_out"),
        "w_rt": np.ascontiguousarray(w_rt), "w_exp_gate": f("w_exp_gate"), "w_exp_up": f("w_exp_up"),
        "w_exp_down": f("w_exp_down"), "w_ple_gate": f("w_ple_gate"), "w_ple_proj": f("w_ple_proj"),
        "par": par, "sA": np.ascontiguousarray(sA), "sP": np.ascontiguousarray(sP), "sC": np.ascontiguousarray(sC),
        "cst": _consts(), "gfin": np.ascontiguousarray(np.broadcast_to(f("g_final")[None, :], (128, D))),
    }
    return shared


def core_inputs(inputs, shared, seqs):
    x = np.asarray(inputs["x"], dtype=np.float32)
    p = np.asarray(inputs["p"], dtype=np.float32)
    pos = np.asarray(inputs["positions"]).astype(np.int32)
    m = dict(shared)
    m["x"] = np.ascontiguousarray(x[seqs])
    m["p"] = np.ascontiguousarray(p[:, seqs])
    m["pos"] = np.ascontiguousarray(pos[seqs].reshape(len(seqs), NT, 128).transpose(0, 2, 1))
    return m


_PROG_CACHE = {}


def kernel(**inputs):
    shared = host_layout(inputs)
    if "full" not in _PROG_CACHE:
        _PROG_CACHE["full"] = build_program(Cfg())[0]
    nc = _PROG_CACHE["full"]
    in_maps = [core_inputs(inputs, shared, [NSEQ * c + i for i in range(NSEQ)]) for c in range(NCORES)]
    res = run_bass_kernel_spmd(nc, in_maps, core_ids=list(range(NCORES)))
    out = np.concatenate([np.asarray(r["out"], dtype=np.float32) for r in res.results], axis=0)
    return out
```
